# Optimizing a Trainium2 kernel written in Bass

```python
import math
import jax
import jax.numpy as jnp
from jax import lax
import numpy as np


D_MODEL = 1024
BATCH = 4
SEQ = 4096
DEPTH = 4

GRID_W = 64
CTX_LEN = 256
N_MIXERS = 3
N_LAYERS_A = (DEPTH + 2) // 3
N_LAYERS_B = (DEPTH + 1) // 3
N_LAYERS_C = DEPTH // 3
N_MOD = 6
EPS = 1e-6

A_HEADS = 8
A_HEAD_DIM = D_MODEL // A_HEADS
A_CHUNK = 64
F_MIN = 1e-30
B_HEADS = 16
B_HEAD_DIM = D_MODEL // B_HEADS
NA_ROWS = 8
NA_COLS = 16
C_GROUP = 16
C_GROUPS = D_MODEL // C_GROUP
C_STATE = 64
D_FF = ((8 * D_MODEL // 3 + 127) // 128) * 128
FFN_CONV = 3

kernel_name = 'hybrid_hgrn2_natten_s5_diffusion_trunk'


def rmsnorm(x, g):
    xf = x.astype(jnp.float32)
    y = xf * lax.rsqrt(jnp.mean(xf * xf, axis=-1, keepdims=True) + EPS)
    return (y * g.astype(jnp.float32)).astype(x.dtype)


def dwconv_centred(h, w, b):
    pad = FFN_CONV // 2
    seq = h.shape[1]
    hp = jnp.pad(h, ((0, 0), (pad, pad), (0, 0)))
    out = b
    for k in range(FFN_CONV):
        out = out + hp[:, k:k + seq] * w[k]
    return out


def conv_ffn(h, w_in, conv_w, conv_b, w_out):
    u = dwconv_centred(h @ w_in, conv_w, conv_b)
    a, v = jnp.split(u, 2, axis=-1)
    return (jax.nn.silu(a) * v) @ w_out


def gla_chunk_scan(q, k, v, logf, s0):
    bsz, seq, heads, dk = q.shape
    dv = v.shape[-1]
    n_chunks = seq // A_CHUNK

    def to_chunks(t):
        return jnp.moveaxis(t.reshape(bsz, n_chunks, A_CHUNK, heads, t.shape[-1]), 1, 0)

    lower_tri = jnp.tril(jnp.ones((A_CHUNK, A_CHUNK), dtype=bool))[None, :, :, None, None]

    def step(state, inp):
        qc, kc, vc, gc = inp
        b = jnp.cumsum(gc, axis=1)
        diff = b[:, :, None] - b[:, None, :]
        decay = jnp.where(lower_tri, jnp.exp(jnp.where(lower_tri, diff, 0.0)), 0.0)
        scores = jnp.einsum('bthk,btshk,bshk->bhts', qc, decay, kc)
        o = jnp.einsum('bhts,bshv->bthv', scores, vc)
        o = o + jnp.einsum('bthk,bhkv->bthv', qc * jnp.exp(b), state)
        b_last = b[:, -1]
        k_dec = kc * jnp.exp(b_last[:, None] - b)
        state = jnp.exp(b_last)[..., None] * state + jnp.einsum('bshk,bshv->bhkv', k_dec, vc)
        return state, o

    s_fin, o = lax.scan(step, s0, (to_chunks(q), to_chunks(k), to_chunks(v), to_chunks(logf)))
    return jnp.moveaxis(o, 0, 1).reshape(bsz, seq, heads, dv), s_fin


def hgrn2_mixer(h_ctx, h_lat, w_in, lower, out_g, w_out):
    lb = lower.reshape(A_HEADS, A_HEAD_DIM).astype(jnp.float32)

    def project(h):
        q, i, g, zf, zb = jnp.split(h @ w_in, 5, axis=-1)
        heads = lambda t: t.reshape(t.shape[0], t.shape[1], A_HEADS, A_HEAD_DIM).astype(jnp.float32)
        return heads(q), heads(i), g, heads(zf), heads(zb)

    pc, pl = project(h_ctx), project(h_lat)
    bsz = h_lat.shape[0]
    s_zero = jnp.zeros((bsz, A_HEADS, A_HEAD_DIM, A_HEAD_DIM), jnp.float32)
    o_ctx, o_lat = [], []
    for gate_idx, rev in ((3, False), (4, True)):
        flip = (lambda t: t[:, ::-1]) if rev else (lambda t: t)

        def run(p, s0):
            f = lb + (1.0 - lb) * jax.nn.sigmoid(p[gate_idx])
            logf = jnp.log(jnp.maximum(f, F_MIN))
            o, s = gla_chunk_scan(flip(p[0]), flip(1.0 - f), flip(p[1]), flip(logf), s0)
            return flip(o), s

        oc, s_ctx = run(pc, s_zero)
        ol, _ = run(pl, s_ctx)
        o_ctx.append(oc)
        o_lat.append(ol)

    def readout(o, g):
        o = o * lax.rsqrt(jnp.mean(o * o, axis=-1, keepdims=True) + EPS)
        o = o.reshape(o.shape[0], o.shape[1], D_MODEL) * out_g.astype(jnp.float32)
        return (o * jax.nn.silu(g.astype(jnp.float32))).astype(g.dtype) @ w_out

    return readout(o_ctx[0] + o_ctx[1], pc[2]), readout(o_lat[0] + o_lat[1], pl[2])


def na_mixer(h_ctx, h_lat, w_qkv, rpb, w_out):
    bsz, seq, _ = h_lat.shape
    rows = seq // GRID_W
    kh, kw = min(NA_ROWS, rows), NA_COLS
    scale = B_HEAD_DIM ** -0.5

    def qkv(h):
        q, k, v = jnp.split(h @ w_qkv, 3, axis=-1)
        heads = lambda t: t.reshape(t.shape[0], t.shape[1], B_HEADS, B_HEAD_DIM)
        return heads(q) * scale, heads(k), heads(v)

    qc, kc, vc = qkv(h_ctx)
    ql, kl, vl = qkv(h_lat)
    p_c = jax.nn.softmax(jnp.einsum('bqhd,bkhd->bhqk', qc, kc).astype(jnp.float32), axis=-1)
    o_c = jnp.einsum('bhqk,bkhd->bqhd', p_c.astype(vc.dtype), vc)
    grid = lambda t: t.reshape(bsz, rows, GRID_W, B_HEADS, B_HEAD_DIM)
    qg, kg, vg = grid(ql), grid(kl), grid(vl)
    col = jnp.arange(GRID_W)
    col_idx = jnp.clip(col - kw // 2, 0, GRID_W - kw)[:, None] + jnp.arange(kw)[None]
    col_bias_idx = col_idx - col[:, None] + NA_COLS - 1
    n_win = kh * kw

    def row_block(r):
        r0 = jnp.clip(r - kh // 2, 0, rows - kh)
        row_bias_idx = r0 + jnp.arange(kh) - r + NA_ROWS - 1
        k_win = lax.dynamic_slice_in_dim(kg, r0, kh, axis=1)[:, :, col_idx]
        v_win = lax.dynamic_slice_in_dim(vg, r0, kh, axis=1)[:, :, col_idx]
        q_r = lax.dynamic_index_in_dim(qg, r, axis=1, keepdims=False)
        bias = rpb[:, row_bias_idx[:, None, None], col_bias_idx[None]]
        s_win = jnp.einsum('bwhd,bjwkhd->bhwjk', q_r, k_win) + jnp.transpose(bias, (0, 2, 1, 3))[None]
        s_ctx = jnp.einsum('bwhd,bchd->bhwc', q_r, kc)
        s = jnp.concatenate([s_win.reshape(bsz, B_HEADS, GRID_W, n_win), s_ctx.astype(s_win.dtype)], axis=-1)
        p = jax.nn.softmax(s.astype(jnp.float32), axis=-1).astype(vc.dtype)
        p_win = p[..., :n_win].reshape(bsz, B_HEADS, GRID_W, kh, kw)
        return (jnp.einsum('bhwjk,bjwkhd->bwhd', p_win, v_win)
                + jnp.einsum('bhwc,bchd->bwhd', p[..., n_win:], vc))

    o_l = jnp.moveaxis(lax.map(row_block, jnp.arange(rows)), 0, 1)
    return o_c.reshape(bsz, -1, D_MODEL) @ w_out, o_l.reshape(bsz, seq, D_MODEL) @ w_out


def s5_mixer(h_ctx, h_lat, lam_re, lam_im, log_dt, b_re, b_im, c_re, c_im, d_skip, w_glu):
    f32 = jnp.float32

    def groups(h):
        return h.reshape(h.shape[0], h.shape[1], C_GROUPS, C_GROUP).astype(f32)

    u_c, u_l = groups(h_ctx), groups(h_lat)
    d = d_skip.reshape(C_GROUPS, C_GROUP).astype(f32)
    y_c, y_l = d * u_c, d * u_l

    def combine(e1, e2):
        a1, b1 = e1
        a2, b2 = e2
        return a1 * a2, a2 * b1 + b2

    def scan(u, a_bar, b_bar, x0):
        bu = jnp.einsum('gpc,blgc->blgp', b_bar, u.astype(jnp.complex64))
        bu = bu.at[:, 0].add(a_bar * x0)
        _, xs = lax.associative_scan(combine, (jnp.broadcast_to(a_bar, bu.shape), bu), axis=1)
        return xs

    x_zero = jnp.zeros((u_l.shape[0], C_GROUPS, C_STATE), jnp.complex64)
    for dirn in range(2):
        flip = (lambda t: t[:, ::-1]) if dirn == 1 else (lambda t: t)
        lam = lax.complex(lam_re[dirn].astype(f32), lam_im[dirn].astype(f32))
        dt = jnp.exp(log_dt[dirn].astype(f32))[:, None]
        a_bar = jnp.exp(lam * dt)
        b_bar = ((a_bar - 1.0) / lam)[..., None] * lax.complex(b_re[dirn].astype(f32), b_im[dirn].astype(f32))
        c_mat = lax.complex(c_re[dirn].astype(f32), c_im[dirn].astype(f32))
        xs_c = scan(flip(u_c), a_bar, b_bar, x_zero)
        xs_l = scan(flip(u_l), a_bar, b_bar, xs_c[:, -1])
        read = lambda xs: flip(jnp.real(jnp.einsum('gcp,blgp->blgc', c_mat, xs)))
        y_c = y_c + read(xs_c)
        y_l = y_l + read(xs_l)

    def glu(y, dtype):
        y = jax.nn.gelu(y.reshape(y.shape[0], y.shape[1], D_MODEL)).astype(dtype)
        a, g = jnp.split(y @ w_glu, 2, axis=-1)
        return a * jax.nn.sigmoid(g)

    return glu(y_c, h_ctx.dtype), glu(y_l, h_lat.dtype)


def setup_inputs(seed: int = 0) -> dict:
    key = jax.random.key(seed)
    ks = iter(jax.random.split(key, 32))
    f32 = jnp.float32
    nrm = lambda shape, s: jax.random.normal(next(ks), shape, f32) * s
    D = D_MODEL
    return {
        'x': nrm((BATCH, SEQ, D), 1.0),
        'c': nrm((BATCH, D), 1.0),
        'ctx': nrm((BATCH, CTX_LEN, D), 1.0),
        'c_ctx': nrm((D,), 1.0),
        'w_mod': nrm((DEPTH, D, N_MOD * D), D ** -0.5),
        'b_mod': nrm((DEPTH, N_MOD * D), 0.01),
        'norm_g': 1.0 + nrm((DEPTH, 4, D), 0.02),
        'a_w_in': nrm((N_LAYERS_A, D, 5 * D), D ** -0.5),
        'a_lower_logits': nrm((DEPTH, D), 0.5),
        'a_out_g': 1.0 + nrm((N_LAYERS_A, D), 0.02),
        'a_w_out': nrm((N_LAYERS_A, D, D), D ** -0.5),
        'b_w_qkv': nrm((N_LAYERS_B, D, 3 * D), D ** -0.5),
        'b_rpb': nrm((N_LAYERS_B, B_HEADS, 2 * NA_ROWS - 1, 2 * NA_COLS - 1), 0.2),
        'b_w_out': nrm((N_LAYERS_B, D, D), D ** -0.5),
        'c_lam_re': -0.5 + nrm((N_LAYERS_C, 2, C_GROUPS, C_STATE), 0.01),
        'c_lam_im': jnp.pi * jnp.arange(C_STATE, dtype=f32) + nrm((N_LAYERS_C, 2, C_GROUPS, C_STATE), 0.01),
        'c_log_dt': jax.random.uniform(next(ks), (N_LAYERS_C, 2, C_GROUPS), f32,
                                       minval=math.log(1e-3), maxval=math.log(1e-1)),
        'c_b_re': nrm((N_LAYERS_C, 2, C_GROUPS, C_STATE, C_GROUP), (2 * C_GROUP) ** -0.5),
        'c_b_im': nrm((N_LAYERS_C, 2, C_GROUPS, C_STATE, C_GROUP), (2 * C_GROUP) ** -0.5),
        'c_c_re': nrm((N_LAYERS_C, 2, C_GROUPS, C_GROUP, C_STATE), (2 * C_STATE) ** -0.5),
        'c_c_im': nrm((N_LAYERS_C, 2, C_GROUPS, C_GROUP, C_STATE), (2 * C_STATE) ** -0.5),
        'c_d': nrm((N_LAYERS_C, D), 1.0),
        'c_w_glu': nrm((N_LAYERS_C, D, 2 * D), D ** -0.5),
        'f_w_in': nrm((DEPTH, D, 2 * D_FF), D ** -0.5),
        'f_conv_w': nrm((DEPTH, FFN_CONV, 2 * D_FF), FFN_CONV ** -0.5),
        'f_conv_b': nrm((DEPTH, 2 * D_FF), 0.01),
        'f_w_out': nrm((DEPTH, D_FF, D), D_FF ** -0.5),
    }


def reference(x, c, ctx, c_ctx, w_mod, b_mod, norm_g, a_w_in, a_lower_logits, a_out_g, a_w_out,
              b_w_qkv, b_rpb, b_w_out, c_lam_re, c_lam_im, c_log_dt, c_b_re, c_b_im, c_c_re, c_c_im,
              c_d, c_w_glu, f_w_in, f_conv_w, f_conv_b, f_w_out):
    p = jax.nn.softmax(a_lower_logits.astype(jnp.float32), axis=0)
    lower = jnp.cumsum(p, axis=0) - p[0]
    lat, cx = x, ctx
    s_lat, s_ctx = jax.nn.silu(c), jax.nn.silu(c_ctx)
    for i in range(DEPTH):
        kind, j = i % N_MIXERS, i // N_MIXERS
        last = i == DEPTH - 1
        m_l = jnp.split((s_lat @ w_mod[i] + b_mod[i])[:, None, :], N_MOD, axis=-1)
        m_c = jnp.split((s_ctx @ w_mod[i] + b_mod[i])[None, None, :], N_MOD, axis=-1)
        h_l = rmsnorm(lat, norm_g[i, 0]) * (1.0 + m_l[1]) + m_l[0]
        h_c = rmsnorm(cx, norm_g[i, 0]) * (1.0 + m_c[1]) + m_c[0]
        if kind == 0:
            y_c, y_l = hgrn2_mixer(h_c, h_l, a_w_in[j], lower[i], a_out_g[j], a_w_out[j])
        elif kind == 1:
            y_c, y_l = na_mixer(h_c, h_l, b_w_qkv[j], b_rpb[j], b_w_out[j])
        else:
            y_c, y_l = s5_mixer(h_c, h_l, c_lam_re[j], c_lam_im[j], c_log_dt[j], c_b_re[j], c_b_im[j],
                                c_c_re[j], c_c_im[j], c_d[j], c_w_glu[j])
        lat = lat + m_l[2] * rmsnorm(y_l, norm_g[i, 1])
        h_l = rmsnorm(lat, norm_g[i, 2]) * (1.0 + m_l[4]) + m_l[3]
        lat = lat + m_l[5] * rmsnorm(conv_ffn(h_l, f_w_in[i], f_conv_w[i], f_conv_b[i], f_w_out[i]), norm_g[i, 3])
        if not last:
            cx = cx + m_c[2] * rmsnorm(y_c, norm_g[i, 1])
            h_c = rmsnorm(cx, norm_g[i, 2]) * (1.0 + m_c[4]) + m_c[3]
            cx = cx + m_c[5] * rmsnorm(conv_ffn(h_c, f_w_in[i], f_conv_w[i], f_conv_b[i], f_w_out[i]), norm_g[i, 3])
    return lat
```

```python
import numpy as np
import concourse.bass as bass
import concourse.mybir as mybir
from concourse.bass_utils import run_bass_kernel_spmd
from contextlib import ExitStack

F32 = mybir.dt.float32
BF16 = mybir.dt.bfloat16
AF = mybir.ActivationFunctionType
ALU = mybir.AluOpType
AX = mybir.AxisListType

ENGS = ('pe', 'act', 'dve', 'pool', 'sp')


class Prog:
    def __init__(self, nc, es, n_dma=(('sp', 40), ('pool', 16), ('act', 8))):
        self.nc = nc
        self.ops = {e: [] for e in ENGS}
        self.esem = {e: es.enter_context(nc.semaphore('s_' + e)) for e in ENGS}
        self.dsem = {}
        self.dval = {}
        self.dnext = {q: 0 for q, _ in n_dma}
        self.dn = dict(n_dma)
        for q, n in n_dma:
            for i in range(n):
                self.dsem[(q, i)] = es.enter_context(nc.semaphore('d_%s%d' % (q, i)))
                self.dval[(q, i)] = 0
        self.lastw = {}
        self.rd_e = {}
        self.rd_d = {}
        self.seen_eng = {e: {f: -1 for f in ENGS} for e in ENGS}
        self.seen_dma = {e: {} for e in ENGS}
        self.ordn = {e: 0 for e in ENGS}
        self.lastop = {e: None for e in ENGS}

    def _need(self, E, tok):
        if tok is None:
            return
        if tok[0] == 'e':
            _, e2, i2 = tok
            if e2 == E:
                if E == 'pe':
                    return
                if self.ordn[E] - self.ops[E][i2][4] > 2:
                    return
                if self.seen_eng[E][E] >= i2:
                    return
            elif self.seen_eng[E][e2] >= i2:
                return
            self.seen_eng[E][e2] = i2
            self.ops[e2][i2][2] = True
            self.ops[E].append(['weng', e2, i2])
        else:
            _, slot, val = tok
            if self.seen_dma[E].get(slot, 0) >= val:
                return
            self.seen_dma[E][slot] = val
            self.ops[E].append(['wdma', slot, val])

    def _deps(self, E, reads, writes):
        for k in reads:
            self._need(E, self.lastw.get(k))
        for k in writes:
            self._need(E, self.lastw.get(k))
            for e2, i2 in self.rd_e.get(k, {}).items():
                self._need(E, ('e', e2, i2))
            for t in self.rd_d.get(k, ()):
                self._need(E, t)

    def _commit(self, tok, reads, writes):
        for k in reads:
            if tok[0] == 'e':
                self.rd_e.setdefault(k, {})[tok[1]] = tok[2]
            else:
                self.rd_d.setdefault(k, []).append(tok)
        for k in writes:
            self.lastw[k] = tok
            self.rd_e[k] = {}
            self.rd_d[k] = []

    def op(self, E, fn, reads=(), writes=()):
        self._deps(E, reads, writes)
        idx = len(self.ops[E])
        self.ops[E].append(['op', fn, False, None, self.ordn[E]])
        self.ordn[E] += 1
        tok = ('e', E, idx)
        self.lastop[E] = tok
        self._commit(tok, reads, writes)
        return tok

    def dma(self, q, out, in_, reads=(), writes=(), **kw):
        slot = (q, self.dnext[q])
        self.dnext[q] = (self.dnext[q] + 1) % self.dn[q]
        prev = self.dval[slot]
        if prev > 0:
            self._need(q, ('d', slot, prev))
        self.dval[slot] = prev + 16
        tok = ('d', slot, prev + 16)
        self._deps(q, reads, writes)
        self.ops[q].append(['op', (lambda e: e.dma_start(out=out, in_=in_, **kw)), False, slot, self.ordn[q]])
        self.ordn[q] += 1
        self._commit(tok, reads, writes)
        return tok

    def barrier(self):
        toks = [self.lastop[e] for e in ENGS if self.lastop[e] is not None]
        dt = [('d', s, v) for s, v in self.dval.items() if v > 0]
        for E in ENGS:
            for t in toks:
                if t[1] != E:
                    self._need(E, t)
            for t in dt:
                self._need(E, t)

    def wait_all_dma(self, E='sp'):
        for s, v in self.dval.items():
            if v > 0:
                self._need(E, ('d', s, v))

    def mm(self, out, lhsT, rhs, start, stop, reads, writes):
        return self.op('pe', lambda e: e.matmul(out, lhsT, rhs, start=start, stop=stop), reads, writes)

    def tr(self, out, in_, ident, reads, writes):
        return self.op('pe', lambda e: e.transpose(out, in_, ident), reads, writes)

    def actf(self, out, in_, func, reads, writes, E='act', **kw):
        return self.op(E, lambda e: e.activation(out, in_, func, **kw), reads, writes)

    def tt(self, E, out, in0, in1, op, reads, writes):
        return self.op(E, lambda e: e.tensor_tensor(out, in0, in1, op), reads, writes)

    def ts(self, E, out, in0, s1, s2, op0, op1, reads, writes):
        if op1 is None:
            return self.op(E, lambda e: e.tensor_scalar(out, in0, s1, 0.0, op0, ALU.add), reads, writes)
        return self.op(E, lambda e: e.tensor_scalar(out, in0, s1, s2, op0, op1), reads, writes)

    def stt(self, E, out, in0, scalar, in1, op0, op1, reads, writes):
        return self.op(E, lambda e: e.scalar_tensor_tensor(out, in0, scalar, in1, op0, op1), reads, writes)

    def cp(self, E, out, in_, reads, writes):
        if E == 'act':
            return self.op(E, lambda e: e.copy(out, in_), reads, writes)
        return self.op(E, lambda e: e.tensor_copy(out, in_), reads, writes)

    def memset(self, E, ap, val, writes):
        return self.op(E, lambda e: e.memset(ap, val), (), writes)

    def emit(self):
        nc = self.nc
        rank = {}
        for e in ENGS:
            r = 0
            for i, ent in enumerate(self.ops[e]):
                if ent[0] == 'op' and ent[2] and ent[3] is None:
                    r += 1
                    rank[(e, i)] = r
        esem, dsem, ops = self.esem, self.dsem, self.ops

        def run(e, eng):
            for i, ent in enumerate(ops[e]):
                if ent[0] == 'op':
                    ins = ent[1](eng)
                    if ent[3] is not None:
                        ins.then_inc(dsem[ent[3]], 16)
                    elif ent[2]:
                        ins.then_inc(esem[e], 1)
                elif ent[0] == 'weng':
                    eng.wait_ge(esem[ent[1]], rank[(ent[1], ent[2])])
                else:
                    eng.wait_ge(dsem[ent[1]], ent[2])

        with nc.Block() as blk:
            blk.tensor(lambda t: run('pe', t))
            blk.scalar(lambda t: run('act', t))
            blk.vector(lambda t: run('dve', t))
            blk.gpsimd(lambda t: run('pool', t))
            blk.sync(lambda t: run('sp', t))
        return {e: len(ops[e]) for e in ENGS}


class Arena:
    def __init__(self, t, nwords):
        self.t = t
        self.n = nwords
        self.off = 0
        self.marks = []

    def alloc(self, shape, dtype=F32):
        n = 1
        for s in shape:
            n *= s
        words = n if dtype == F32 else (n + 1) // 2
        words = (words + 7) // 8 * 8
        assert self.off + words <= self.n, ('SBUF arena overflow', self.off, words, self.n)
        ap = self.t[:, self.off:self.off + words]
        self.off += words
        if dtype != F32:
            ap = ap.bitcast(dtype)
        ap = ap[:, 0:n]
        if len(shape) > 1:
            names = ['d%d' % i for i in range(len(shape))]
            pat = 'p (' + ' '.join(names) + ') -> p ' + ' '.join(names)
            ap = ap.rearrange(pat, **{names[i]: shape[i] for i in range(1, len(shape))})
        return ap

    def mark(self):
        self.marks.append(self.off)

    def release(self):
        self.off = self.marks.pop()


def simulate(P):
    rank = {}
    for e in ENGS:
        r = 0
        for i, ent in enumerate(P.ops[e]):
            if ent[0] == 'op' and ent[2] and ent[3] is None:
                r += 1
                rank[(e, i)] = r
    es = {e: 0 for e in ENGS}
    ds = {s: 0 for s in P.dsem}
    pc = {e: 0 for e in ENGS}
    while True:
        prog = False
        done = True
        for e in ENGS:
            while pc[e] < len(P.ops[e]):
                ent = P.ops[e][pc[e]]
                if ent[0] == 'op':
                    if ent[3] is not None:
                        ds[ent[3]] += 16
                    elif ent[2]:
                        es[e] += 1
                elif ent[0] == 'weng':
                    if es[ent[1]] < rank[(ent[1], ent[2])]:
                        break
                else:
                    if ds[ent[1]] < ent[2]:
                        break
                pc[e] += 1
                prog = True
            if pc[e] < len(P.ops[e]):
                done = False
        if done:
            return True
        if not prog:
            raise RuntimeError('DEADLOCK at ' + str({e: (pc[e], P.ops[e][pc[e]] if pc[e] < len(P.ops[e]) else None) for e in ENGS}))


D = 1024
KC = 8
NCX = 256
NLAT = 4096
NT = NCX + NLAT
DFF = 2816
DEPTH = 4
EPS = 1e-6
SEQS = ((0, NCX), (NCX, NLAT))

PV = {}
_o = 0
for _n, _c in (('ng', 128), ('bmod', 192), ('alog', 32), ('aog', 16), ('cd', 8), ('fcw', 528), ('fcb', 176)):
    PV[_n] = _o
    _o += _c
NPV = _o


def fm(v):
    return np.ascontiguousarray(np.asarray(v, np.float32).reshape(-1, 128).T)


class Ctx:
    pass


def setup_phase(P, A, g):
    nc = g.nc
    g.pvt = A.alloc([NPV])
    g.cvt = A.alloc([8, 2])
    g.sil = A.alloc([8, 2])
    g.identf = A.alloc([128])
    g.identb = A.alloc([128], BF16)
    g.onesb = A.alloc([128], BF16)
    g.mod = A.alloc([4, 48, 2])
    g.der = A.alloc([4, 6, 8, 2])
    g.lowr = A.alloc([2, 8])
    g.one_m_lowr = A.alloc([2, 8])
    P.dma('sp', g.pvt, g.pv_d, (), ['pvt'])
    P.dma('sp', g.cvt, g.cv_d, (), ['cvt'])
    P.dma('sp', g.identf, g.ident_d, (), ['identf'])
    P.cp('dve', g.identb, g.identf, ['identf'], ['identb'])
    P.memset('pool', g.onesb, 1.0, ['onesb'])
    P.actf(g.sil, g.cvt, AF.Silu, ['cvt'], ['sil'])
    A.mark()
    wm = [A.alloc([8, 1024]) for _ in range(2)]
    psm = g.ps[:, 0, 0:96].rearrange('p (a b) -> p a b', b=2)
    cnt = 0
    for l in range(DEPTH):
        for pc in range(6):
            b = cnt % 2
            cnt += 1
            P.dma('sp' if cnt % 2 else 'pool', wm[b], g.w_mod[l].rearrange('(k p) n -> p k n', p=128)[:, :, pc * 1024:(pc + 1) * 1024],
                  (), [('wm', b)])
            for jj in range(8):
                j = pc * 8 + jj
                for k in range(KC):
                    P.mm(psm[:, j, :], wm[b][:, k, jj * 128:(jj + 1) * 128], g.sil[:, k, :], k == 0, k == KC - 1,
                         [('wm', b), 'sil'], ['psm'])
        for v in range(2):
            P.tt('dve', g.mod[:, l, :, v], psm[:, :, v], g.pvt[:, PV['bmod'] + l * 48:PV['bmod'] + (l + 1) * 48], ALU.add,
                 ['psm', 'pvt'], ['mod'])

        def ng(i):
            o = PV['ng'] + (l * 4 + i) * 8
            return g.pvt[:, o:o + 8]
        for v in range(2):
            m = lambda w: g.mod[:, l, w * 8:(w + 1) * 8, v]
            P.stt('dve', g.der[:, l, 0, :, v], m(1), 1.0, ng(0), ALU.add, ALU.mult, ['mod', 'pvt'], ['der'])
            P.cp('dve', g.der[:, l, 1, :, v], m(0), ['mod'], ['der'])
            P.tt('dve', g.der[:, l, 2, :, v], m(2), ng(1), ALU.mult, ['mod', 'pvt'], ['der'])
            P.stt('dve', g.der[:, l, 3, :, v], m(4), 1.0, ng(2), ALU.add, ALU.mult, ['mod', 'pvt'], ['der'])
            P.cp('dve', g.der[:, l, 4, :, v], m(3), ['mod'], ['der'])
            P.tt('dve', g.der[:, l, 5, :, v], m(5), ng(3), ALU.mult, ['mod', 'pvt'], ['der'])
    al = lambda l: g.pvt[:, PV['alog'] + l * 8:PV['alog'] + (l + 1) * 8]
    tmp = A.alloc([6, 8])
    P.tt('dve', tmp[:, 4, :], al(0), al(1), ALU.max, ['pvt'], ['lt'])
    P.tt('dve', tmp[:, 5, :], al(2), al(3), ALU.max, ['pvt'], ['lt'])
    P.tt('dve', tmp[:, 4, :], tmp[:, 4, :], tmp[:, 5, :], ALU.max, ['lt'], ['lt'])
    for l in range(4):
        P.tt('dve', tmp[:, l, :], al(l), tmp[:, 4, :], ALU.subtract, ['pvt', 'lt'], ['lt'])
    P.actf(tmp[:, 0:4, :], tmp[:, 0:4, :], AF.Exp, ['lt'], ['lt'])
    P.tt('dve', tmp[:, 4, :], tmp[:, 1, :], tmp[:, 2, :], ALU.add, ['lt'], ['lt'])
    P.tt('dve', tmp[:, 4, :], tmp[:, 4, :], tmp[:, 3, :], ALU.add, ['lt'], ['lt'])
    P.tt('dve', tmp[:, 5, :], tmp[:, 4, :], tmp[:, 0, :], ALU.add, ['lt'], ['lt'])
    P.op('dve', lambda e: e.reciprocal(tmp[:, 5, :], tmp[:, 5, :]), ['lt'], ['lt'])
    P.memset('dve', g.lowr[:, 0, :], 0.0, ['lowr'])
    P.tt('dve', g.lowr[:, 1, :], tmp[:, 4, :], tmp[:, 5, :], ALU.mult, ['lt'], ['lowr'])
    P.ts('dve', g.one_m_lowr, g.lowr, -1.0, 1.0, ALU.mult, ALU.add, ['lowr'], ['omlowr'])
    P.barrier()
    A.release()


def transpose_in(P, A, g, R):
    A.mark()
    xt = [A.alloc([D]) for _ in range(2)]
    xT = [A.alloc([KC, 128]) for _ in range(2)]
    Rv = R[1].rearrange('(c p) n -> p c n', p=128)
    for t in range(NT // 128):
        b = t % 2
        P.dma('sp', xt[b], g.xin[t * 128:(t + 1) * 128, :], (), [('xt', b)])
        for hlf in range(2):
            pst = g.ps[:, 2 * b + hlf, :].rearrange('p (a b) -> p a b', b=128)
            for c in range(4):
                cc = hlf * 4 + c
                P.tr(pst[:, c, :], xt[b][:, cc * 128:(cc + 1) * 128], g.identf, [('xt', b), 'identf'], [('pst', b, hlf)])
            P.cp('act' if hlf else 'dve', xT[b][:, hlf * 4:(hlf + 1) * 4, :], pst, [('pst', b, hlf)], [('xT', b)])
        P.dma('sp', Rv[:, :, t * 128:(t + 1) * 128], xT[b], [('xT', b)], [('R', R[0], t * 128 // 256)])
    P.barrier()
    A.release()


def transpose_out(P, A, g, R):
    A.mark()
    xT = [A.alloc([KC, 128]) for _ in range(2)]
    xt = [A.alloc([D]) for _ in range(2)]
    Rv = R[1].rearrange('(c p) n -> p c n', p=128)
    for t in range(NLAT // 128):
        b = t % 2
        t0 = NCX + t * 128
        P.dma('sp', xT[b], Rv[:, :, t0:t0 + 128], [('R', R[0], t0 // 256)], [('oxT', b)])
        for hlf in range(2):
            pst = g.ps[:, 2 * b + hlf, :].rearrange('p (a b) -> p a b', b=128)
            for c in range(4):
                cc = hlf * 4 + c
                P.tr(pst[:, c, :], xT[b][:, cc, :], g.identf, [('oxT', b), 'identf'], [('pst', b, hlf)])
            P.cp('act' if hlf else 'dve', xt[b][:, hlf * 512:(hlf + 1) * 512], g.ps[:, 2 * b + hlf, :], [('pst', b, hlf)], [('oxt', b)])
        P.dma('sp', g.out[t * 128:(t + 1) * 128, :], xt[b], [('oxt', b)], [('out', t)])
    P.barrier()
    A.release()


def load_weight_bf16(P, A, g, dst, src_ap, kchunks, ncols, key, stg, stgkey):
    sv = src_ap.rearrange('(k p) n -> p k n', p=128)
    i = 0
    for k in range(kchunks):
        for c0 in range(0, ncols, 1024):
            c1 = min(ncols, c0 + 1024)
            b = g.stg_cnt % 2
            g.stg_cnt += 1
            P.dma('sp' if b else 'act', stg[b][:, 0:c1 - c0], sv[:, k, c0:c1], (), [(stgkey, b)])
            P.cp('pool', dst[:, k, c0:c1], stg[b][:, 0:c1 - c0], [(stgkey, b)], [key])
            i += 1


def norm_tile(P, g, src, n, Acol, Bcol, out, sq, rstd, psn, rk, wk, sqk, tmp, tk):
    P.actf(sq[:, :, 0:n], src, AF.Square, rk, [sqk])
    for c in range(KC):
        P.mm(psn[:, 0:n], g.onesb, sq[:, c, 0:n], c == 0, c == KC - 1, ['onesb', sqk], ['psn'])
    P.actf(rstd[:, 0:n], psn[:, 0:n], AF.Sqrt, ['psn'], ['rstd'], scale=1.0 / D, bias=EPS)
    P.op('dve', lambda e: e.reciprocal(rstd[:, 0:n], rstd[:, 0:n]), ['rstd'], ['rstd'])
    for c in range(KC):
        P.stt('dve', tmp[:, c, 0:n], src[:, c, :], Acol[:, c:c + 1], rstd[:, 0:n], ALU.mult, ALU.mult,
              rk + ['rstd', 'der'], [(tk, c)])
        P.actf(out[:, c, :], tmp[:, c, 0:n], AF.Identity, [(tk, c), 'der'], wk, bias=Bcol[:, c:c + 1], scale=1.0)


def ffn_phase(P, A, g, l, Rin, Rout, last):
    A.mark()
    win = A.alloc([KC, 2 * DFF], BF16)
    wout = A.alloc([22, D], BF16)
    stg = [A.alloc([1024]) for _ in range(2)]
    load_weight_bf16(P, A, g, win, g.f_w_in[l], KC, 2 * DFF, 'win', stg, 'stg')
    load_weight_bf16(P, A, g, wout, g.f_w_out[l], 22, D, 'wout', stg, 'stg')
    NB = 256
    W = NB + 2
    src = A.alloc([KC, W])
    hT = A.alloc([KC, W], BF16)
    sq = A.alloc([KC, W], BF16)
    ybuf = A.alloc([KC, W])
    rstd = A.alloc([W])
    acc = [A.alloc([NB]) for _ in range(4)]
    sa = [A.alloc([NB]) for _ in range(2)]
    gv = A.alloc([22, NB], BF16)
    y = ybuf[:, :, 0:NB]
    ysq = sq[:, :, 0:NB]
    ro = [A.alloc([KC, NB]) for _ in range(1)]
    Rvi = Rin[1].rearrange('(c p) n -> p c n', p=128)
    Rvo = Rout[1].rearrange('(c p) n -> p c n', p=128)
    psn = g.ps[:, 0, :]
    fcw = lambda kk, j: g.pvt[:, PV['fcw'] + (l * 3 + kk) * 44 + j:PV['fcw'] + (l * 3 + kk) * 44 + j + 1]
    fcb = lambda j: g.pvt[:, PV['fcb'] + l * 44 + j:PV['fcb'] + l * 44 + j + 1]
    ti = 0
    for v, (base, ln) in ((1, SEQS[0]), (0, SEQS[1])):
        if last and v == 1:
            continue
        A2 = g.der[:, l, 3, :, v]
        B2 = g.der[:, l, 4, :, v]
        G2 = g.der[:, l, 5, :, v]
        for s in range(0, ln, NB):
            lo, hi = s - 1, s + NB + 1
            vlo, vhi = max(lo, 0), min(hi, ln)
            rkeys = [('R', Rin[0], (base + t) // 256) for t in sorted(set([vlo, s, vhi - 1]))]
            P.dma('sp', src[:, :, vlo - lo:vhi - lo], Rvi[:, :, base + vlo:base + vhi], rkeys, ['src'])
            if vlo > lo:
                P.memset('pool', src[:, :, 0:1], 0.0, ['src'])
            if vhi < hi:
                P.memset('pool', src[:, :, W - 1:W], 0.0, ['src'])
            norm_tile(P, g, src, W, A2, B2, hT, sq, rstd, psn, ['src'], ['hT'], 'sq', ybuf, 'y')
            if vlo > lo:
                P.memset('pool', hT[:, :, 0:1], 0.0, ['hT'])
            if vhi < hi:
                P.memset('pool', hT[:, :, W - 1:W], 0.0, ['hT'])
            for jj in range(22):
                for half in range(2):
                    j = jj + 22 * half
                    pb = 1 + (2 * jj + half) % 4
                    pu = g.ps[:, pb, 0:W]
                    ac = acc[(2 * jj + half) % 4]
                    ak = ('acc', (2 * jj + half) % 4)
                    for k in range(KC):
                        P.mm(pu, win[:, k, j * 128:(j + 1) * 128], hT[:, k, :], k == 0, k == KC - 1, ['win', 'hT'], [('pu', pb)])
                    P.actf(ac, pu[:, 1:NB + 1], AF.Identity, [('pu', pb), 'pvt'], [ak], scale=fcw(1, j), bias=fcb(j))
                    P.stt('dve', ac, pu[:, 0:NB], fcw(0, j), ac, ALU.mult, ALU.add, [('pu', pb), 'pvt', ak], [ak])
                    P.stt('dve', ac, pu[:, 2:NB + 2], fcw(2, j), ac, ALU.mult, ALU.add, [('pu', pb), 'pvt', ak], [ak])
                    if half == 0:
                        P.actf(sa[jj % 2], ac, AF.Silu, [ak], [('sa', jj % 2)])
                    else:
                        P.tt('pool', gv[:, jj, :], sa[jj % 2], ac, ALU.mult, [('sa', jj % 2), ak], ['gv'])
            for m in range(KC):
                pb = 5 + m % 2
                py = g.ps[:, pb, 0:NB]
                for jj in range(22):
                    P.mm(py, wout[:, jj, m * 128:(m + 1) * 128], gv[:, jj, :], jj == 0, jj == 21, ['wout', 'gv'], [('py', pb)])
                P.cp('act', y[:, m, :], py, [('py', pb)], [('y', m)])
            yk = [('y', m) for m in range(KC)]
            P.actf(ysq, y, AF.Square, yk, ['sq'])
            for c in range(KC):
                P.mm(psn[:, 0:NB], g.onesb, ysq[:, c, :], c == 0, c == KC - 1, ['onesb', 'sq'], ['psn'])
            P.actf(rstd[:, 0:NB], psn[:, 0:NB], AF.Sqrt, ['psn'], ['rstd'], scale=1.0 / D, bias=EPS)
            P.op('dve', lambda e: e.reciprocal(rstd[:, 0:NB], rstd[:, 0:NB]), ['rstd'], ['rstd'])
            rb = ro[0]
            rbk = ('ro', 0)
            for c in range(KC):
                P.stt('dve', y[:, c, :], y[:, c, :], G2[:, c:c + 1], rstd[:, 0:NB], ALU.mult, ALU.mult, [('y', c), 'rstd', 'der'], [('y', c)])
                P.tt('pool', rb[:, c, :], y[:, c, :], src[:, c, 1:NB + 1], ALU.add, [('y', c), 'src'], [rbk])
            P.dma('sp', Rvo[:, :, base + s:base + s + NB], rb, [rbk], [('R', Rout[0], (base + s) // 256)])
            ti += 1
    P.barrier()
    A.release()


def copy_R(P, A, g, Rin, Rout, lo, hi):
    P.dma('sp', Rout[1][:, lo:hi], Rin[1][:, lo:hi], [('R', Rin[0], t) for t in range(lo // 256, (hi + 255) // 256)],
          [('R', Rout[0], t) for t in range(lo // 256, (hi + 255) // 256)])


TILES = [(0, NCX, 1)] + [(NCX + 512 * i, 512, 0) for i in range(8)]


def hgrn_phase(P, A, g, l, Rin, Rout, last):
    jw = l // 3
    li = 0 if l == 0 else 1
    lb = g.lowr[:, li, :]
    oml = g.one_m_lowr[:, li, :]
    fmv = lambda d: d.rearrange('(c p) n -> p c n', p=128)
    Rvi = fmv(Rin[1])
    Rvo = fmv(Rout[1])
    A.mark()
    win = A.alloc([KC, 5120], BF16)
    stg = [A.alloc([1024]) for _ in range(2)]
    load_weight_bf16(P, A, g, win, g.a_w_in[jw], KC, 5120, 'win', stg, 'stg')
    src = A.alloc([KC, 512])
    hT = A.alloc([KC, 512], BF16)
    sq = A.alloc([KC, 512], BF16)
    tmp = A.alloc([KC, 512])
    rstd = A.alloc([512])
    obs = [A.alloc([KC, 512]) for _ in range(2)]
    vb = A.alloc([4, 1024], BF16)
    ew = [A.alloc([512]) for _ in range(2)]
    psn = g.ps[:, 0, :]
    oc = 0
    pc = 0
    for (t0, n, v) in TILES:
        A1 = g.der[:, l, 0, :, v]
        B1 = g.der[:, l, 1, :, v]
        P.dma('sp', src[:, :, 0:n], Rvi[:, :, t0:t0 + n], [('R', Rin[0], t) for t in range(t0 // 256, (t0 + n) // 256)], ['src'])
        norm_tile(P, g, src[:, :, 0:n], n, A1, B1, hT[:, :, 0:n], sq, rstd, psn, ['src'], ['hT'], 'sq', tmp, 'tmp')
        for part, dst in ((0, g.Qd), (2, g.Gd)):
            ob = obs[oc % 2]
            obk = ('ob', oc % 2)
            oc += 1
            for c in range(KC):
                pb = 1 + pc % 4
                pc += 1
                pp = g.ps[:, pb, 0:n]
                for k in range(KC):
                    P.mm(pp, win[:, k, part * 1024 + c * 128:part * 1024 + (c + 1) * 128], hT[:, k, 0:n], k == 0, k == KC - 1,
                         ['win', 'hT'], [('pp', pb)])
                P.cp('act' if c % 2 else 'dve', ob[:, c, 0:n], pp, [('pp', pb)], [obk])
            P.dma('sp', fmv(dst)[:, :, t0:t0 + n], ob[:, :, 0:n], [obk], [])
        for d in range(2):
            kb = obs[oc % 2]
            kbk = ('ob', oc % 2)
            oc += 1
            lfb = obs[oc % 2]
            lfk = ('ob', oc % 2)
            oc += 1
            for c in range(KC):
                pb = 1 + pc % 4
                pc += 1
                pp = g.ps[:, pb, 0:n]
                col = (3 + d) * 1024 + c * 128
                for k in range(KC):
                    P.mm(pp, win[:, k, col:col + 128], hT[:, k, 0:n], k == 0, k == KC - 1, ['win', 'hT'], [('pp', pb)])
                e0 = ew[c % 2]
                ek = ('ew', c % 2)
                P.actf(e0[:, 0:n], pp, AF.Exp, [('pp', pb)], [ek], scale=-1.0)
                P.ts('dve', e0[:, 0:n], e0[:, 0:n], 1e30, 1.0, ALU.min, ALU.add, [ek], [ek])
                P.op('dve', lambda e, e0=e0, n=n: e.reciprocal(e0[:, 0:n], e0[:, 0:n]), [ek], [ek])
                P.ts('dve', e0[:, 0:n], e0[:, 0:n], oml[:, c:c + 1], lb[:, c:c + 1], ALU.mult, ALU.add, [ek, 'omlowr', 'lowr'], [ek])
                P.ts('pool', kb[:, c, 0:n], e0[:, 0:n], -1.0, 1.0, ALU.mult, ALU.add, [ek], [kbk])
                P.actf(lfb[:, c, 0:n], e0[:, 0:n], AF.Ln, [ek], [lfk])
            P.dma('sp', fmv(g.Kd[d])[:, :, t0:t0 + n], kb[:, :, 0:n], [kbk], [])
            P.dma('sp', fmv(g.LFd[d])[:, :, t0:t0 + n], lfb[:, :, 0:n], [lfk], [])
        for ts_ in range(n // 128):
            for half in range(2):
                pb = 5 + (ts_ * 2 + half) % 2
                pv_ = g.ps[:, pb, :]
                for k in range(KC):
                    P.mm(pv_, hT[:, k, ts_ * 128:(ts_ + 1) * 128], win[:, k, 1024 + half * 512:1024 + (half + 1) * 512], k == 0, k == KC - 1,
                         ['win', 'hT'], [('pv', pb)])
                P.cp('act' if half else 'dve', vb[:, ts_, half * 512:(half + 1) * 512], pv_, [('pv', pb)], ['vb'])
        P.dma('sp', g.Vd[t0:t0 + n, :].rearrange('(s p) f -> p s f', p=128), vb[:, 0:n // 128, :], ['vb'], [])
    P.barrier()
    A.release()

    A.mark()
    CH = 32
    HB = 16
    NCM = 512 // CH
    hc = A.alloc([3, 8 * CH])
    P.dma('sp', hc, g.hconst, (), ['hc'])
    rst = hc[:, 0, :]
    St = A.alloc([8, 128])
    Sb = A.alloc([8, 128], BF16)
    qs = A.alloc([NCM, 8, CH])
    ks = A.alloc([NCM, 8, CH])
    lfs = A.alloc([NCM, 8, CH])
    os_ = A.alloc([NCM, 8, CH])
    vs = A.alloc([NCM, 1024], BF16)
    W3 = lambda x: x.rearrange('p (h t) -> p h t', t=CH)
    cf = A.alloc([8 * CH])
    d1f = A.alloc([8 * CH])
    d2f = A.alloc([8 * CH])
    tpf = A.alloc([8 * CH])
    E = [A.alloc([8 * CH]) for _ in range(5)]
    eend = A.alloc([8])
    qh = A.alloc([8, CH], BF16)
    qS = A.alloc([8, HB], BF16)
    kf = A.alloc([8, CH], BF16)
    k1 = A.alloc([8, CH], BF16)
    kh = A.alloc([8, CH], BF16)
    pt = A.alloc([8, CH], BF16)
    kt = A.alloc([8, 128], BF16)
    ps_sc = g.ps[0:CH, 1, 0:8 * CH].rearrange('p (h t) -> p h t', t=CH)
    ps_kt = g.ps[0:CH, 2, :].bitcast(BF16).rearrange('p (h k) -> p h k', k=128)
    ps_o = g.ps[:, 3, 0:8 * CH].rearrange('p (h t) -> p h t', t=CH)
    ps_s = [g.ps[:, 4, :].rearrange('p (h v) -> p h v', v=128), g.ps[:, 5, :].rearrange('p (h v) -> p h v', v=128)]
    c3, d13, d23, tp3 = W3(cf), W3(d1f), W3(d2f), W3(tpf)
    E3 = [W3(x) for x in E]
    for d in range(2):
        P.memset('dve', St, 0.0, ['St'])
        P.memset('pool', Sb, 0.0, ['Sb'])
        P.memset('pool', kf, 0.0, ['kf'])
        mask = hc[0:CH, 1 + d, :].rearrange('p (h t) -> p h t', t=CH)
        order = TILES if d == 0 else [TILES[0]] + TILES[:0:-1]
        Fs = slice(0, HB) if d == 0 else slice(HB, CH)
        Ss = slice(HB, CH) if d == 0 else slice(0, HB)
        imid = HB - 1 if d == 0 else HB
        iend = CH - 1 if d == 0 else 0
        for (t0, n, v) in order:
            nch = n // CH
            cv = lambda dd, h: dd[h * 128:(h + 1) * 128, t0:t0 + n].rearrange('p (c t) -> p c t', t=CH)
            for h in range(8):
                P.dma('sp', qs[:, 0:nch, h, :], cv(g.Qd, h), (), ['qs'])
                P.dma('act', ks[:, 0:nch, h, :], cv(g.Kd[d], h), (), ['ks'])
                P.dma('pool', lfs[:, 0:nch, h, :], cv(g.LFd[d], h), (), ['lfs'])
            P.dma('act', vs[0:CH, 0:nch, :], g.Vd[t0:t0 + n, :].rearrange('(c p) f -> p c f', p=CH), (), ['vs'])
            for ci in (range(nch) if d == 0 else range(nch - 1, -1, -1)):
                q3 = qs[:, ci]
                k3 = ks[:, ci]
                lf3 = lfs[:, ci]
                lf2 = lf3.rearrange('p h t -> p (h t)')
                if d == 0:
                    P.op('dve', lambda e, lf2=lf2: e.tensor_tensor_scan(cf, rst, lf2, 0.0, ALU.mult, ALU.add), ['hc', 'lfs'], ['c'])
                else:
                    P.op('dve', lambda e, lf2=lf2: e.tensor_tensor_scan(tpf, rst, lf2, 0.0, ALU.mult, ALU.add), ['hc', 'lfs'], ['tp'])
                    P.tt('pool', d13, lf3, tp3, ALU.subtract, ['lfs', 'tp'], ['d1'])
                    P.tt('dve', c3, d13, tp3[:, :, CH - 1:CH].to_broadcast([128, 8, CH]), ALU.add, ['d1', 'tp'], ['c'])
                P.tt('dve', d13, c3, c3[:, :, imid:imid + 1].to_broadcast([128, 8, CH]), ALU.subtract, ['c'], ['d1'])
                P.tt('pool', d23, c3, c3[:, :, iend:iend + 1].to_broadcast([128, 8, CH]), ALU.subtract, ['c'], ['d2'])
                P.actf(E3[0], c3, AF.Exp, ['c'], ['E0'])
                P.actf(E3[1][:, :, Ss], d13[:, :, Ss], AF.Exp, ['d1'], ['E1'])
                P.actf(E3[2][:, :, Fs], c3[:, :, Fs], AF.Exp, ['c'], ['E2'], scale=-1.0)
                P.actf(E3[3], d13, AF.Exp, ['d1'], ['E3'], scale=-1.0)
                P.actf(E3[4], d23, AF.Exp, ['d2'], ['E4'], scale=-1.0)
                P.actf(eend, c3[:, :, iend], AF.Exp, ['c'], ['eend'])
                P.tt('dve', qh, q3, E3[0], ALU.mult, ['qs', 'E0'], ['qh'])
                P.tt('pool', qS, q3[:, :, Ss], E3[1][:, :, Ss], ALU.mult, ['qs', 'E1'], ['qS'])
                P.stt('dve', kf[:, :, Fs], E3[2][:, :, Fs], 1e30, k3[:, :, Fs], ALU.min, ALU.mult, ['ks', 'E2'], ['kf'])
                P.stt('dve', k1, E3[3], 1e30, k3, ALU.min, ALU.mult, ['ks', 'E3'], ['k1'])
                P.tt('pool', kh, k3, E3[4], ALU.mult, ['ks', 'E4'], ['kh'])
                for h in range(8):
                    P.mm(ps_sc[:, h, Ss], k1[:, h, :], qS[:, h, :], True, True, ['k1', 'qS'], ['ps_sc'])
                    P.mm(ps_sc[:, h, Fs], kf[:, h, :], qh[:, h, Fs], True, True, ['kf', 'qh'], ['ps_sc'])
                for h in range(8):
                    P.tr(ps_kt[:, h, :], kh[:, h, :], g.identb, ['kh', 'identb'], ['ps_kt'])
                P.tt('dve', pt[0:CH], ps_sc, mask, ALU.mult, ['ps_sc', 'hc'], ['pt'])
                P.cp('act', kt[0:CH], ps_kt, ['ps_kt'], ['kt'])
                for h in range(8):
                    vch = vs[0:CH, ci, h * 128:(h + 1) * 128]
                    P.mm(ps_o[:, h, :], vch, pt[0:CH, h, :], True, False, ['vs', 'pt'], ['ps_o'])
                    P.mm(ps_o[:, h, :], Sb[:, h, :], qh[:, h, :], False, True, ['Sb', 'qh'], ['ps_o'])
                for h in range(8):
                    vch = vs[0:CH, ci, h * 128:(h + 1) * 128]
                    P.mm(ps_s[h // 4][:, h % 4, :], kt[0:CH, h, :], vch, True, True, ['kt', 'vs'], ['ps_s'])
                P.cp('act', os_[:, ci], ps_o, ['ps_o'], ['os'])
                for h in range(8):
                    P.stt('dve', St[:, h, :], St[:, h, :], eend[:, h:h + 1], ps_s[h // 4][:, h % 4, :], ALU.mult, ALU.add,
                          ['St', 'eend', 'ps_s'], ['St'])
                P.cp('pool', Sb, St, ['St'], ['Sb'])
            for h in range(8):
                P.dma('sp', cv(g.Od[d], h), os_[:, 0:nch, h, :], ['os'], [])
    P.barrier()
    A.release()

    A.mark()
    wout = A.alloc([KC, D], BF16)
    stg = [A.alloc([1024]) for _ in range(2)]
    load_weight_bf16(P, A, g, wout, g.a_w_out[jw], KC, D, 'wout', stg, 'stg')
    src = A.alloc([KC, 512])
    of = A.alloc([KC, 512])
    obk_ = A.alloc([KC, 512])
    gg = A.alloc([KC, 512])
    osq = A.alloc([KC, 512], BF16)
    zT = A.alloc([KC, 512], BF16)
    y = A.alloc([KC, 512])
    rstd = A.alloc([512])
    rh = [A.alloc([512]) for _ in range(2)]
    psn = g.ps[:, 0, :]
    aog = lambda c: g.pvt[:, PV['aog'] + jw * 8 + c:PV['aog'] + jw * 8 + c + 1]
    for (t0, n, v) in TILES:
        if last and v == 1:
            continue
        G1 = g.der[:, l, 2, :, v]
        P.dma('sp', src[:, :, 0:n], Rvi[:, :, t0:t0 + n], [('R', Rin[0], t) for t in range(t0 // 256, (t0 + n) // 256)], ['src'])
        P.dma('act', of[:, :, 0:n], fmv(g.Od[0])[:, :, t0:t0 + n], (), ['of'])
        P.dma('sp', obk_[:, :, 0:n], fmv(g.Od[1])[:, :, t0:t0 + n], (), ['obk'])
        P.dma('act', gg[:, :, 0:n], fmv(g.Gd)[:, :, t0:t0 + n], (), ['gg'])
        P.tt('pool', of[:, :, 0:n], of[:, :, 0:n], obk_[:, :, 0:n], ALU.add, ['of', 'obk'], ['of'])
        P.actf(osq[:, :, 0:n], of[:, :, 0:n], AF.Square, ['of'], ['osq'])
        P.actf(gg[:, :, 0:n], gg[:, :, 0:n], AF.Silu, ['gg'], ['gg'])
        for h in range(8):
            pb = 1 + h % 2
            pr = g.ps[:, pb, 0:n]
            r_ = rh[h % 2]
            rk_ = ('rh', h % 2)
            P.mm(pr, g.onesb, osq[:, h, 0:n], True, True, ['onesb', 'osq'], [('pr', pb)])
            P.actf(r_[:, 0:n], pr, AF.Sqrt, [('pr', pb)], [rk_], scale=1.0 / 128, bias=EPS)
            P.op('dve', lambda e, r_=r_, n=n: e.reciprocal(r_[:, 0:n], r_[:, 0:n]), [rk_], [rk_])
            P.stt('dve', of[:, h, 0:n], of[:, h, 0:n], aog(h), r_[:, 0:n], ALU.mult, ALU.mult, ['of', 'pvt', rk_], ['of'])
            P.tt('pool', zT[:, h, 0:n], of[:, h, 0:n], gg[:, h, 0:n], ALU.mult, ['of', 'gg'], ['zT'])
        for m in range(KC):
            pb = 3 + m % 2
            py = g.ps[:, pb, 0:n]
            for h in range(8):
                P.mm(py, wout[:, h, m * 128:(m + 1) * 128], zT[:, h, 0:n], h == 0, h == 7, ['wout', 'zT'], [('py', pb)])
            P.cp('act', y[:, m, 0:n], py, [('py', pb)], [('y', m)])
        post_norm_store(P, g, y, n, G1, src, osq, rstd, psn, Rvo, Rout, t0)
    if last:
        pass
    P.barrier()
    A.release()


def post_norm_store(P, g, y, n, G, src, sqb, rstd, psn, Rvo, Rout, t0):
    yk = [('y', m) for m in range(KC)]
    P.actf(sqb[:, :, 0:n], y[:, :, 0:n], AF.Square, yk, ['osq'])
    for c in range(KC):
        P.mm(psn[:, 0:n], g.onesb, sqb[:, c, 0:n], c == 0, c == KC - 1, ['onesb', 'osq'], ['psn'])
    P.actf(rstd[:, 0:n], psn[:, 0:n], AF.Sqrt, ['psn'], ['rstd'], scale=1.0 / D, bias=EPS)
    P.op('dve', lambda e: e.reciprocal(rstd[:, 0:n], rstd[:, 0:n]), ['rstd'], ['rstd'])
    for c in range(KC):
        P.stt('dve', y[:, c, 0:n], y[:, c, 0:n], G[:, c:c + 1], rstd[:, 0:n], ALU.mult, ALU.mult, [('y', c), 'rstd', 'der'], [('y', c)])
        P.tt('pool', y[:, c, 0:n], y[:, c, 0:n], src[:, c, 0:n], ALU.add, [('y', c), 'src'], [('y', c)])
    P.dma('sp', Rvo[:, :, t0:t0 + n], y[:, :, 0:n], yk, [('R', Rout[0], t) for t in range(t0 // 256, (t0 + n) // 256)])


NEG = -30000.0


def na_host_bias(rpb):
    out = np.full((16, 5, 128, 640), NEG, np.float32)
    ql = np.arange(128)
    rl, w = ql // 64, ql % 64
    kk = np.arange(640)
    jj, c = kk // 64, kk % 64
    for pat, r in enumerate((8, 0, 2, 60, 62)):
        R0 = min(max(r - 4, 0), 54)
        qrow = (r + rl)[:, None]
        r0 = np.clip(qrow - 4, 0, 56)
        c0 = np.clip(w - 8, 0, 48)[:, None]
        keyrow = (R0 + jj)[None, :]
        cc = c[None, :]
        valid = (keyrow >= r0) & (keyrow < r0 + 8) & (cc >= c0) & (cc < c0 + 16)
        ri = np.clip(keyrow - qrow + 7, 0, 14)
        ci = np.clip(cc - w[:, None] + 15, 0, 30)
        vals = rpb[:, ri, ci]
        out[:, pat] = np.where(valid[None], vals, NEG)
    return out


def na_phase(P, A, g, l, Rin, Rout, last):
    fmv = lambda d: d.rearrange('(c p) n -> p c n', p=128)
    Rvi = fmv(Rin[1])
    Rvo = fmv(Rout[1])
    A.mark()
    win = A.alloc([KC, 3072], BF16)
    stg = [A.alloc([1024]) for _ in range(2)]
    load_weight_bf16(P, A, g, win, g.b_w_qkv[0], KC, 3072, 'win', stg, 'stg')
    src = A.alloc([KC, 512])
    hT = A.alloc([KC, 512], BF16)
    sq = A.alloc([KC, 512], BF16)
    tmp = A.alloc([KC, 512])
    rstd = A.alloc([512])
    obs = [A.alloc([KC, 512], BF16) for _ in range(2)]
    vb = A.alloc([4, 1024], BF16)
    psn = g.ps[:, 0, :]
    oc = 0
    pc = 0
    for (t0, n, v) in TILES:
        A1 = g.der[:, l, 0, :, v]
        B1 = g.der[:, l, 1, :, v]
        P.dma('sp', src[:, :, 0:n], Rvi[:, :, t0:t0 + n], [('R', Rin[0], t) for t in range(t0 // 256, (t0 + n) // 256)], ['src'])
        norm_tile(P, g, src[:, :, 0:n], n, A1, B1, hT[:, :, 0:n], sq, rstd, psn, ['src'], ['hT'], 'sq', tmp, 'tmp')
        for part in range(2):
            ob = obs[oc % 2]
            obk = ('ob', oc % 2)
            oc += 1
            for c in range(KC):
                pb = 1 + pc % 4
                pc += 1
                pp = g.ps[:, pb, 0:n]
                for k in range(KC):
                    P.mm(pp, win[:, k, part * 1024 + c * 128:part * 1024 + (c + 1) * 128], hT[:, k, 0:n], k == 0, k == KC - 1,
                         ['win', 'hT'], [('pp', pb)])
                P.cp('act' if c % 2 else 'dve', ob[:, c, 0:n], pp, [('pp', pb)], [obk])
            P.dma('sp', fmv(g.QKd[part])[:, :, t0:t0 + n], ob[:, :, 0:n], [obk], [])
        for ts_ in range(n // 128):
            for half in range(2):
                pb = 5 + (ts_ * 2 + half) % 2
                pv_ = g.ps[:, pb, :]
                for k in range(KC):
                    P.mm(pv_, hT[:, k, ts_ * 128:(ts_ + 1) * 128], win[:, k, 2048 + half * 512:2048 + (half + 1) * 512], k == 0, k == KC - 1,
                         ['win', 'hT'], [('pv', pb)])
                P.cp('act' if half else 'dve', vb[:, ts_, half * 512:(half + 1) * 512], pv_, [('pv', pb)], ['vb'])
        P.dma('sp', g.Vd[t0:t0 + n, :].rearrange('(s p) f -> p s f', p=128), vb[:, 0:n // 128, :], ['vb'], [])
    P.barrier()
    A.release()

    A.mark()
    qh = [A.alloc([NT], BF16) for _ in range(2)]
    kh = [A.alloc([NT], BF16) for _ in range(2)]
    vh = [A.alloc([NT // 128, 64], BF16) for _ in range(2)]
    bh = [A.alloc([5, 640]) for _ in range(2)]
    oh = [A.alloc([NT], BF16) for _ in range(2)]
    sm = [A.alloc([896]) for _ in range(2)]
    pe_ = [A.alloc([896]) for _ in range(2)]
    pn = [A.alloc([896], BF16) for _ in range(2)]
    pT = [A.alloc([7, 128], BF16) for _ in range(2)]
    st = [A.alloc([4]) for _ in range(2)]
    blocks = [('c', 0), ('c', 1)] + [('l', r) for r in range(0, 64, 2)]
    bi = 0
    for h in range(16):
        hb = h % 2
        P.dma('sp', qh[hb][0:64], g.QKd[0][h * 64:(h + 1) * 64, :], (), [('qh', hb)])
        P.dma('act', kh[hb][0:64], g.QKd[1][h * 64:(h + 1) * 64, :], (), [('kh', hb)])
        P.dma('pool', vh[hb], g.Vd[:, h * 64:(h + 1) * 64].rearrange('(s p) f -> p s f', p=128), (), [('vh', hb)])
        P.dma('sp', bh[hb], g.nbias[h].rearrange('a p k -> p a k'), (), [('bh', hb)])
        for (kind, r) in blocks:
            b = bi % 2
            bi += 1
            ps_a = g.ps[:, 2 * b, :]
            ps_b = g.ps[:, 2 * b + 1, :]
            ps_t = g.ps[:, 4 + b, 0:448].bitcast(BF16).rearrange('p (i q) -> p i q', q=128)
            ps_o = g.ps[0:64, 6 + b, 0:128]
            smb, peb, pnb, pTb, stb = sm[b], pe_[b], pn[b], pT[b], st[b]
            if kind == 'c':
                qt = r * 128
                lo = 640
                tiles_i = [5, 6]
                vt = {5: 0, 6: 1}
            else:
                qt = NCX + r * 64
                R0 = min(max(r - 4, 0), 54)
                k0 = NCX + R0 * 64
                pat = {0: 1, 2: 2, 60: 3, 62: 4}.get(r, 0)
                lo = 0
                tiles_i = list(range(7))
                vt = {i: 2 + R0 // 2 + i for i in range(5)}
                vt[5] = 0
                vt[6] = 1
            qap = qh[hb][0:64, qt:qt + 128]
            if kind == 'l':
                P.mm(ps_a, qap, kh[hb][0:64, k0:k0 + 512], True, True, [('qh', hb), ('kh', hb)], [('psa', b)])
                P.mm(ps_b[:, 0:128], qap, kh[hb][0:64, k0 + 512:k0 + 640], True, True, [('qh', hb), ('kh', hb)], [('psb', b)])
            P.mm(ps_b[:, 128:384], qap, kh[hb][0:64, 0:NCX], True, True, [('qh', hb), ('kh', hb)], [('psb', b)])
            if kind == 'l':
                P.stt('dve', smb[:, 0:512], ps_a, 0.125, bh[hb][:, pat, 0:512], ALU.mult, ALU.add, [('psa', b), ('bh', hb)], [('sm', b)])
                P.stt('dve', smb[:, 512:640], ps_b[:, 0:128], 0.125, bh[hb][:, pat, 512:640], ALU.mult, ALU.add,
                      [('psb', b), ('bh', hb)], [('sm', b)])
            P.actf(smb[:, 640:896], ps_b[:, 128:384], AF.Copy, [('psb', b)], [('sm', b)], scale=0.125)
            P.op('dve', lambda e, stb=stb, smb=smb, lo=lo: e.reduce_max(stb[:, 0:1], smb[:, lo:896], AX.X), [('sm', b)], [('st', b)])
            P.ts('dve', stb[:, 1:2], stb[:, 0:1], -1.0, None, ALU.mult, None, [('st', b)], [('st', b)])
            P.memset('pool', stb[:, 2:3], 0.0, [('st2', b)])
            P.actf(peb[:, lo:896], smb[:, lo:896], AF.Exp, [('sm', b), ('st', b), ('st2', b)], [('pe', b), ('st2', b)],
                   bias=stb[:, 1:2], scale=1.0, accum_out=stb[:, 2:3])
            P.op('dve', lambda e, stb=stb: e.reciprocal(stb[:, 3:4], stb[:, 2:3]), [('st2', b)], [('st3', b)])
            P.ts('dve', pnb[:, lo:896], peb[:, lo:896], stb[:, 3:4], None, ALU.mult, None, [('pe', b), ('st3', b)], [('pn', b)])
            for i in tiles_i:
                P.tr(ps_t[:, i, :], pnb[:, i * 128:(i + 1) * 128], g.identb, [('pn', b), 'identb'], [('pst', b)])
            i0 = tiles_i[0]
            P.cp('act', pTb[:, i0:7, :], ps_t[:, i0:7, :], [('pst', b)], [('pT', b)])
            for n_, i in enumerate(tiles_i):
                P.mm(ps_o, vh[hb][:, vt[i], :], pTb[:, i, :], n_ == 0, n_ == len(tiles_i) - 1, [('vh', hb), ('pT', b)], [('pso', b)])
            P.cp('act', oh[hb][0:64, qt:qt + 128], ps_o, [('pso', b)], [('oh', hb)])
        P.dma('sp', g.Ond[h], oh[hb][0:64], [('oh', hb)], [])
    P.barrier()
    A.release()

    A.mark()
    wout = A.alloc([16, D], BF16)
    stg = [A.alloc([1024]) for _ in range(2)]
    sv = g.b_w_out[0].rearrange('(k p) n -> p k n', p=64)
    for k in range(16):
        b = k % 2
        P.dma('sp', stg[b][0:64], sv[:, k, :], (), [('stg', b)])
        P.cp('pool', wout[0:64, k, :], stg[b][0:64], [('stg', b)], ['wout'])
    src = A.alloc([KC, 512])
    oT = A.alloc([16, 512], BF16)
    osq = A.alloc([KC, 512], BF16)
    y = A.alloc([KC, 512])
    rstd = A.alloc([512])
    psn = g.ps[:, 0, :]
    for (t0, n, v) in TILES:
        if last and v == 1:
            continue
        G1 = g.der[:, l, 2, :, v]
        P.dma('sp', src[:, :, 0:n], Rvi[:, :, t0:t0 + n], [('R', Rin[0], t) for t in range(t0 // 256, (t0 + n) // 256)], ['src'])
        P.dma('act', oT[0:64, :, 0:n], g.Ond[:, :, t0:t0 + n].rearrange('h p n -> p h n'), (), ['oT'])
        for m in range(KC):
            pb = 3 + m % 2
            py = g.ps[:, pb, 0:n]
            for h in range(16):
                P.mm(py, wout[0:64, h, m * 128:(m + 1) * 128], oT[0:64, h, 0:n], h == 0, h == 15, ['wout', 'oT'], [('py', pb)])
            P.cp('act', y[:, m, 0:n], py, [('py', pb)], [('y', m)])
        post_norm_store(P, g, y, n, G1, src, osq, rstd, psn, Rvo, Rout, t0)
    P.barrier()
    A.release()


ST = 128
import math as _m


def s5_host(inp):
    lre = np.asarray(inp['c_lam_re'], np.float32)[0]
    lim = np.asarray(inp['c_lam_im'], np.float32)[0]
    ldt = np.asarray(inp['c_log_dt'], np.float32)[0]
    Bre = np.asarray(inp['c_b_re'], np.float32)[0]
    Bim = np.asarray(inp['c_b_im'], np.float32)[0]
    Cre = np.asarray(inp['c_c_re'], np.float32)[0]
    Cim = np.asarray(inp['c_c_im'], np.float32)[0]
    sm_ = lambda a: np.ascontiguousarray(a.reshape(32, 128).T)
    s5p = np.zeros((2, 128, 3, 32), np.float32)
    Bblk = np.zeros((2, 2, 128, 8, 512), np.float32)
    Cblk = np.zeros((2, 2, 128, 32, 128), np.float32)
    for d in range(2):
        s5p[d, :, 0] = sm_(lre[d])
        s5p[d, :, 1] = sm_(lim[d])
        s5p[d, :, 2] = sm_(np.repeat(ldt[d][:, None], 64, axis=1))
        for gi in range(64):
            ch, gloc = gi // 8, gi % 8
            sic, gl2 = gloc // 2, gloc % 2
            s = gi // 2
            for ri, (Bm, Cm) in enumerate(((Bre, Cre), (Bim, Cim))):
                Bblk[d, ri, gloc * 16:(gloc + 1) * 16, ch, sic * 128 + gl2 * 64:sic * 128 + gl2 * 64 + 64] = Bm[d, gi].T
                Cblk[d, ri, gl2 * 64:(gl2 + 1) * 64, s, gloc * 16:(gloc + 1) * 16] = Cm[d, gi].T
    return {'s5p': s5p, 'Bblk': Bblk, 'Cblk': Cblk}


def s5_phase(P, A, g, l, Rin, Rout, last):
    fmv = lambda d: d.rearrange('(c p) n -> p c n', p=128)
    Rvi = fmv(Rin[1])
    Rvo = fmv(Rout[1])
    T = ST
    A.mark()
    src = A.alloc([KC, 512])
    hT = [A.alloc([KC, 512], BF16) for _ in range(2)]
    sq = A.alloc([KC, 512], BF16)
    tmp = A.alloc([KC, 512])
    rstd = A.alloc([512])
    psn = g.ps[:, 0, :]
    for i, (t0, n, v) in enumerate(TILES):
        A1 = g.der[:, l, 0, :, v]
        B1 = g.der[:, l, 1, :, v]
        P.dma('sp', src[:, :, 0:n], Rvi[:, :, t0:t0 + n], [('R', Rin[0], t) for t in range(t0 // 256, (t0 + n) // 256)], ['src'])
        norm_tile(P, g, src[:, :, 0:n], n, A1, B1, hT[i % 2][:, :, 0:n], sq, rstd, psn, ['src'], [('hT', i % 2)], 'sq', tmp, 'tmp')
        P.dma('sp', fmv(g.Hd)[:, :, t0:t0 + n], hT[i % 2][:, :, 0:n], [('hT', i % 2)], [])
    P.barrier()
    A.release()

    chunks_f = [(c * T) for c in range(NT // T)]
    chunks_b = [T, 0] + [c * T for c in range(NT // T - 1, 1, -1)]
    for d in range(2):
        A.mark()
        Ct = A.alloc([32, T])
        St_ = A.alloc([32, T])
        Rm = A.alloc([32, T])
        bbre = A.alloc([8, 512], BF16)
        bbim = A.alloc([8, 512], BF16)
        Cre = A.alloc([32, 128], BF16)
        Cimn = A.alloc([32, 128], BF16)
        rr = A.alloc([32])
        xp = A.alloc([2, 32])
        A.mark()
        prm = A.alloc([3, 32])
        sc = [A.alloc([32]) for _ in range(12)]
        P.dma('sp', prm, g.s5p[d], (), ['prm'])
        lre, lim = prm[:, 0, :], prm[:, 1, :]
        dt, ar, th, cc, ss, t1, t2, are, aim, nre, nim, den = sc
        K_ = ['s5s']
        P.actf(dt, prm[:, 2, :], AF.Exp, ['prm'], K_)
        P.tt('dve', ar, lre, dt, ALU.mult, ['prm'] + K_, K_)
        P.tt('dve', th, lim, dt, ALU.mult, ['prm'] + K_, K_)
        P.actf(rr, ar, AF.Exp, K_, ['rr'])
        P.actf(ss, th, AF.Sin, K_, K_, scale=1.0 / 32)
        P.actf(cc, th, AF.Sin, K_, K_, scale=1.0 / 32, bias=_m.pi / 2)
        for _ in range(5):
            P.tt('dve', t1, cc, cc, ALU.mult, K_, K_)
            P.tt('dve', t2, ss, ss, ALU.mult, K_, K_)
            P.stt('dve', ss, cc, 2.0, ss, ALU.mult, ALU.mult, K_, K_)
            P.tt('dve', cc, t1, t2, ALU.subtract, K_, K_)
        P.tt('dve', are, rr, cc, ALU.mult, K_ + ['rr'], K_)
        P.tt('dve', aim, rr, ss, ALU.mult, K_ + ['rr'], K_)
        P.ts('dve', t1, are, -1.0, None, ALU.add, None, K_, K_)
        P.tt('dve', nre, t1, lre, ALU.mult, K_ + ['prm'], K_)
        P.tt('dve', t2, aim, lim, ALU.mult, K_ + ['prm'], K_)
        P.tt('dve', nre, nre, t2, ALU.add, K_, K_)
        P.tt('dve', nim, aim, lre, ALU.mult, K_ + ['prm'], K_)
        P.tt('dve', t2, t1, lim, ALU.mult, K_ + ['prm'], K_)
        P.tt('dve', nim, nim, t2, ALU.subtract, K_, K_)
        P.tt('dve', den, lre, lre, ALU.mult, ['prm'] + K_, K_)
        P.tt('dve', t2, lim, lim, ALU.mult, ['prm'] + K_, K_)
        P.tt('dve', den, den, t2, ALU.add, K_, K_)
        P.op('dve', lambda e, den=den: e.reciprocal(den, den), K_, K_)
        P.tt('dve', nre, nre, den, ALU.mult, K_, K_)
        P.tt('dve', nim, nim, den, ALU.mult, K_, K_)
        P.cp('dve', Ct[:, :, 0], cc, K_, ['tab'])
        P.cp('dve', St_[:, :, 0], ss, K_, ['tab'])
        ta = A.alloc([32, T // 2])
        tb = A.alloc([32, T // 2])
        k = 1
        while k < T:
            br = Ct[:, :, k - 1:k].to_broadcast([128, 32, k])
            bi = St_[:, :, k - 1:k].to_broadcast([128, 32, k])
            P.tt('dve', ta[:, :, 0:k], Ct[:, :, 0:k], br, ALU.mult, ['tab'], ['ta'])
            P.tt('pool', tb[:, :, 0:k], St_[:, :, 0:k], bi, ALU.mult, ['tab'], ['tb'])
            P.tt('dve', ta[:, :, 0:k], ta[:, :, 0:k], tb[:, :, 0:k], ALU.subtract, ['ta', 'tb'], ['ta'])
            P.tt('pool', tb[:, :, 0:k], Ct[:, :, 0:k], bi, ALU.mult, ['tab', 'ta'], ['tb'])
            P.cp('dve', Ct[:, :, k:2 * k], ta[:, :, 0:k], ['ta', 'tb'], ['tab'])
            P.tt('dve', ta[:, :, 0:k], St_[:, :, 0:k], br, ALU.mult, ['tab'], ['ta'])
            P.tt('dve', St_[:, :, k:2 * k], ta[:, :, 0:k], tb[:, :, 0:k], ALU.add, ['ta', 'tb', 'tab'], ['tab'])
            k *= 2
        P.cp('dve', Rm, rr[:, :, None].to_broadcast([128, 32, T]) if False else rr.unsqueeze(2).to_broadcast([128, 32, T]), ['rr'], ['Rm'])
        P.memset('dve', Rm[:, :, 0:1], 0.0, ['Rm'])
        P.dma('sp', g.cfd[d, 0].rearrange('s n -> n s'), nre, K_, ['cfd'], allow_slow_non_contiguous=True)
        P.dma('sp', g.cfd[d, 1].rearrange('s n -> n s'), nim, K_, ['cfd'], allow_slow_non_contiguous=True)
        cbr = A.alloc([2048])
        cbi = A.alloc([2048])
        Bs = [A.alloc([2048]) for _ in range(2)]
        o1 = A.alloc([2048])
        o2 = A.alloc([2048])
        f2 = lambda x: x.rearrange('p c n -> p (c n)')
        for hf in range(2):
            cs = slice(hf * 2048, (hf + 1) * 2048)
            P.dma('sp', cbr, g.cfd[d, 0].rearrange('s n -> (s n)')[cs].partition_broadcast(128), ['cfd'], ['cbr'])
            P.dma('sp', cbi, g.cfd[d, 1].rearrange('s n -> (s n)')[cs].partition_broadcast(128), ['cfd'], ['cbi'])
            P.dma('act', Bs[0], g.Bblk[d, 0].rearrange('p c n -> p (c n)')[:, cs], (), ['Bs0'])
            P.dma('act', Bs[1], g.Bblk[d, 1].rearrange('p c n -> p (c n)')[:, cs], (), ['Bs1'])
            P.tt('dve', o1, cbr, Bs[0], ALU.mult, ['cbr', 'Bs0'], ['o1'])
            P.tt('pool', o2, cbi, Bs[1], ALU.mult, ['cbi', 'Bs1'], ['o2'])
            P.tt('dve', f2(bbre)[:, cs], o1, o2, ALU.subtract, ['o1', 'o2'], ['bbre'])
            P.tt('dve', o1, cbr, Bs[1], ALU.mult, ['cbr', 'Bs1', 'bbre'], ['o1'])
            P.tt('pool', o2, cbi, Bs[0], ALU.mult, ['cbi', 'Bs0', 'bbre'], ['o2'])
            P.tt('dve', f2(bbim)[:, cs], o1, o2, ALU.add, ['o1', 'o2'], ['bbim'])
        for hf in range(2):
            cs = slice(hf * 2048, (hf + 1) * 2048)
            P.dma('sp', Bs[0], g.Cblk[d, 0].rearrange('p s n -> p (s n)')[:, cs], ['o1', 'o2'], ['Bs0'])
            P.dma('act', Bs[1], g.Cblk[d, 1].rearrange('p s n -> p (s n)')[:, cs], ['o1', 'o2'], ['Bs1'])
            P.cp('act', Cre.rearrange('p s n -> p (s n)')[:, cs], Bs[0], ['Bs0'], ['Cre'])
            P.op('act', lambda e, Cimn=Cimn, Bs=Bs, cs=cs: e.mul(Cimn.rearrange('p s n -> p (s n)')[:, cs], Bs[1], -1.0), ['Bs1'], ['Cimn'])
        P.barrier()
        A.release()
        uT = [A.alloc([KC, T], BF16) for _ in range(2)]
        wre = A.alloc([4, T])
        wim = A.alloc([4, T])
        tq = A.alloc([4, T])
        xr = A.alloc([4, T])
        xi = A.alloc([4, T])
        Xb = A.alloc([32, 2, T], BF16)
        ysb = [A.alloc([KC, T]) for _ in range(2)]
        cin = A.alloc([2, 32])
        P.memset('dve', xp, 0.0, ['xp'])
        flat = lambda x: x.rearrange('p s t -> p (s t)')
        for ci, t0 in enumerate(chunks_f if d == 0 else chunks_b):
            if t0 == NCX and d == 0:
                pass
            ub = uT[ci % 2]
            ubk = ('uT', ci % 2)
            P.dma('sp', ub, fmv(g.Hd)[:, :, t0:t0 + T], (), [ubk])
            P.tt('dve', cin[:, 0, :], rr, xp[:, 0, :], ALU.mult, ['rr', 'xp'], ['cin'])
            P.tt('dve', cin[:, 1, :], rr, xp[:, 1, :], ALU.mult, ['rr', 'xp'], ['cin'])
            for gq in range(8):
                pre = g.ps[:, 1 + 2 * (gq % 2), :].rearrange('p (s t) -> p s t', t=T)
                pim = g.ps[:, 2 + 2 * (gq % 2), :].rearrange('p (s t) -> p s t', t=T)
                pk = ('pbu', gq % 2)
                for sic in range(4):
                    ru = ub[:, gq, :] if d == 0 else ub[:, gq, ::-1]
                    P.mm(pre[:, sic, :], bbre[:, gq, sic * 128:(sic + 1) * 128], ru, True, True, ['bbre', ubk], [pk])
                    P.mm(pim[:, sic, :], bbim[:, gq, sic * 128:(sic + 1) * 128], ru, True, True, ['bbim', ubk], [pk])
                s0 = gq * 4
                C4, S4 = Ct[:, s0:s0 + 4, :], St_[:, s0:s0 + 4, :]
                P.tt('dve', wre, pre, C4, ALU.mult, [pk, 'tab'], ['wre'])
                P.tt('dve', tq, pim, S4, ALU.mult, [pk, 'tab'], ['tq'])
                P.tt('pool', wre, wre, tq, ALU.add, ['wre', 'tq'], ['wre'])
                P.tt('dve', wim, pim, C4, ALU.mult, [pk, 'tab'], ['wim'])
                P.tt('dve', tq, pre, S4, ALU.mult, [pk, 'tab', 'wre'], ['tq'])
                P.tt('pool', wim, wim, tq, ALU.subtract, ['wim', 'tq'], ['wim'])
                P.tt('pool', wre[:, :, 0], wre[:, :, 0], cin[:, 0, s0:s0 + 4], ALU.add, ['wre', 'cin'], ['wre'])
                P.tt('pool', wim[:, :, 0], wim[:, :, 0], cin[:, 1, s0:s0 + 4], ALU.add, ['wim', 'cin'], ['wim'])
                R4 = flat(Rm[:, s0:s0 + 4, :])
                P.op('dve', lambda e, R4=R4: e.tensor_tensor_scan(flat(wre), R4, flat(wre), 0.0, ALU.mult, ALU.add), ['wre', 'Rm'], ['wre'])
                P.op('dve', lambda e, R4=R4: e.tensor_tensor_scan(flat(wim), R4, flat(wim), 0.0, ALU.mult, ALU.add), ['wim', 'Rm'], ['wim'])
                P.tt('dve', xr, wre, C4, ALU.mult, ['wre', 'tab'], ['xr'])
                P.tt('pool', tq, wim, S4, ALU.mult, ['wim', 'tab'], ['tq'])
                P.tt('dve', xr, xr, tq, ALU.subtract, ['xr', 'tq'], ['xr'])
                P.tt('pool', xi, wre, S4, ALU.mult, ['wre', 'tab'], ['xi'])
                P.tt('dve', tq, wim, C4, ALU.mult, ['wim', 'tab', 'xr'], ['tq'])
                P.tt('pool', xi, xi, tq, ALU.add, ['xi', 'tq'], ['xi'])
                P.cp('act', Xb[:, s0:s0 + 4, 0, :], xr, ['xr'], ['Xb'])
                P.cp('act', Xb[:, s0:s0 + 4, 1, :], xi, ['xi'], ['Xb'])
                P.cp('act', xp[:, 0, s0:s0 + 4], xr[:, :, T - 1], ['xr'], ['xp'])
                P.cp('act', xp[:, 1, s0:s0 + 4], xi[:, :, T - 1], ['xi'], ['xp'])
            yb = ysb[ci % 2]
            ybk = ('ysb', ci % 2)
            for ch in range(8):
                py = g.ps[:, 5 + (ch // 4), :].rearrange('p (c t) -> p c t', t=T)[:, ch % 4, :]
                n_ = 0
                for sic in range(4):
                    s = ch * 4 + sic
                    for ri, Cw in enumerate((Cre, Cimn)):
                        rx = Xb[:, s, ri, :] if d == 0 else Xb[:, s, ri, ::-1]
                        P.mm(py, Cw[:, s, :], rx, n_ == 0, n_ == 7, ['Cre', 'Cimn', 'Xb'], [('py5', ch // 4)])
                        n_ += 1
            for hf in range(2):
                P.cp('act', yb[:, hf * 4:(hf + 1) * 4, :], g.ps[:, 5 + hf, :].rearrange('p (c t) -> p c t', t=T), [('py5', hf)], [ybk])
            P.dma('sp', fmv(g.Yd[d])[:, :, t0:t0 + T], yb, [ybk], [])
        P.barrier()
        A.release()

    A.mark()
    wgl = A.alloc([KC, 2048], BF16)
    stg = [A.alloc([1024]) for _ in range(2)]
    load_weight_bf16(P, A, g, wgl, g.c_w_glu[0], KC, 2048, 'wgl', stg, 'stg')
    src = A.alloc([KC, 512])
    hb = A.alloc([KC, 512], BF16)
    yf = A.alloc([KC, 512])
    yb_ = A.alloc([KC, 512])
    t3 = A.alloc([KC, 512])
    gl = A.alloc([KC, 512], BF16)
    osq = A.alloc([KC, 512], BF16)
    y = A.alloc([KC, 512])
    sg = [A.alloc([512]) for _ in range(2)]
    rstd = A.alloc([512])
    psn = g.ps[:, 0, :]
    cd = lambda c: g.pvt[:, PV['cd'] + c:PV['cd'] + c + 1]
    for (t0, n, v) in TILES:
        if last and v == 1:
            continue
        G1 = g.der[:, l, 2, :, v]
        P.dma('sp', src[:, :, 0:n], Rvi[:, :, t0:t0 + n], [('R', Rin[0], t) for t in range(t0 // 256, (t0 + n) // 256)], ['src'])
        P.dma('act', hb[:, :, 0:n], fmv(g.Hd)[:, :, t0:t0 + n], (), ['hb'])
        P.dma('sp', yf[:, :, 0:n], fmv(g.Yd[0])[:, :, t0:t0 + n], (), ['yf'])
        P.dma('act', yb_[:, :, 0:n], fmv(g.Yd[1])[:, :, t0:t0 + n], (), ['yb'])
        P.tt('pool', yf[:, :, 0:n], yf[:, :, 0:n], yb_[:, :, 0:n], ALU.add, ['yf', 'yb'], ['yf'])
        for c in range(KC):
            P.stt('dve', yf[:, c, 0:n], hb[:, c, 0:n], cd(c), yf[:, c, 0:n], ALU.mult, ALU.add, ['hb', 'pvt', 'yf'], ['yf'])
        P.actf(t3[:, :, 0:n], yf[:, :, 0:n], AF.Square, ['yf'], ['t3'])
        P.ts('dve', t3[:, :, 0:n], t3[:, :, 0:n], 0.044715, 1.0, ALU.mult, ALU.add, ['t3'], ['t3'])
        P.tt('pool', t3[:, :, 0:n], t3[:, :, 0:n], yf[:, :, 0:n], ALU.mult, ['t3', 'yf'], ['t3'])
        P.actf(t3[:, :, 0:n], t3[:, :, 0:n], AF.Sigmoid, ['t3'], ['t3'], scale=2.0 * _m.sqrt(2.0 / _m.pi))
        P.tt('dve', gl[:, :, 0:n], t3[:, :, 0:n], yf[:, :, 0:n], ALU.mult, ['t3', 'yf'], ['gl'])
        for m in range(KC):
            pa = g.ps[:, 1 + 2 * (m % 2), 0:n]
            pg = g.ps[:, 2 + 2 * (m % 2), 0:n]
            pk = ('pag', m % 2)
            for k in range(KC):
                P.mm(pa, wgl[:, k, m * 128:(m + 1) * 128], gl[:, k, 0:n], k == 0, k == KC - 1, ['wgl', 'gl'], [pk])
            for k in range(KC):
                P.mm(pg, wgl[:, k, D + m * 128:D + (m + 1) * 128], gl[:, k, 0:n], k == 0, k == KC - 1, ['wgl', 'gl'], [pk])
            P.actf(sg[m % 2][:, 0:n], pg, AF.Sigmoid, [pk], [('sg', m % 2)])
            P.tt('dve', y[:, m, 0:n], pa, sg[m % 2][:, 0:n], ALU.mult, [pk, ('sg', m % 2)], [('y', m)])
        post_norm_store(P, g, y, n, G1, src, osq, rstd, psn, Rvo, Rout, t0)
    P.barrier()
    A.release()


def extra_inputs(nc, g):
    g.hconst = nc.dram_tensor("hconst", [128, 3, 256], F32, kind="ExternalInput").ap()
    g.nbias = nc.dram_tensor("nbias", [16, 5, 128, 640], F32, kind="ExternalInput").ap()
    g.s5p = nc.dram_tensor("s5p", [2, 128, 3, 32], F32, kind="ExternalInput").ap()
    g.Bblk = nc.dram_tensor("Bblk", [2, 2, 128, 8, 512], F32, kind="ExternalInput").ap()
    g.Cblk = nc.dram_tensor("Cblk", [2, 2, 128, 32, 128], F32, kind="ExternalInput").ap()


def extra_scratch(nc, g):
    g.Qd = nc.dram_tensor("Qd", [D, NT], F32).ap()
    g.Gd = nc.dram_tensor("Gd", [D, NT], F32).ap()
    g.Kd = [nc.dram_tensor("Kd%d" % d, [D, NT], F32).ap() for d in range(2)]
    g.LFd = [nc.dram_tensor("LFd%d" % d, [D, NT], F32).ap() for d in range(2)]
    g.Od = [nc.dram_tensor("Od%d" % d, [D, NT], F32).ap() for d in range(2)]
    g.Vd = nc.dram_tensor("Vd", [NT, D], BF16).ap()
    g.QKd = [nc.dram_tensor("QKd%d" % d, [D, NT], BF16).ap() for d in range(2)]
    g.Ond = nc.dram_tensor("Ond", [16, 64, NT], BF16).ap()
    g.Hd = nc.dram_tensor("Hd", [D, NT], BF16).ap()
    g.Yd = [nc.dram_tensor("Yd%d" % d, [D, NT], F32).ap() for d in range(2)]
    g.cfd = nc.dram_tensor("cfd", [2, 2, 32, 128], F32).ap()


def extra_host(inp):
    CH = 32
    hc = np.zeros((128, 3, 8 * CH), np.float32)
    r = np.ones((8, CH), np.float32)
    r[:, 0] = 0.0
    hc[:, 0, :] = r.reshape(-1)[None, :]
    s = np.arange(CH)[:, None]
    t = np.arange(CH)[None, :]
    hc[0:CH, 1, :] = np.tile((s <= t).astype(np.float32), (1, 8))
    hc[0:CH, 2, :] = np.tile((s >= t).astype(np.float32), (1, 8))
    r = {'hconst': hc, 'nbias': na_host_bias(np.asarray(inp['b_rpb'], np.float32)[0])}
    r.update(s5_host(inp))
    return r


def mixer_phase(P, A, g, l, Rin, Rout, last):
    kind = l % 3
    if kind == 0:
        hgrn_phase(P, A, g, l, Rin, Rout, last)
    elif kind == 1:
        na_phase(P, A, g, l, Rin, Rout, last)
    else:
        s5_phase(P, A, g, l, Rin, Rout, last)


W_NAMES = ['w_mod', 'a_w_in', 'a_w_out', 'b_w_qkv', 'b_w_out', 'c_w_glu', 'f_w_in', 'f_w_out']
W_SHAPES = {'w_mod': [4, 1024, 6144], 'a_w_in': [2, 1024, 5120], 'a_w_out': [2, 1024, 1024], 'b_w_qkv': [1, 1024, 3072],
            'b_w_out': [1, 1024, 1024], 'c_w_glu': [1, 1024, 2048], 'f_w_in': [4, 1024, 5632], 'f_w_out': [4, 2816, 1024]}


def build(plan=None, dbg_out=None):
    nc = bass.Bass("TRN2", target_bir_lowering=False)
    g = Ctx()
    g.nc = nc
    g.xin = nc.dram_tensor("xin", [NT, D], F32, kind="ExternalInput").ap()
    g.pv_d = nc.dram_tensor("pv", [128, NPV], F32, kind="ExternalInput").ap()
    g.cv_d = nc.dram_tensor("cv", [128, 8, 2], F32, kind="ExternalInput").ap()
    g.ident_d = nc.dram_tensor("ident", [128, 128], F32, kind="ExternalInput").ap()
    for n in W_NAMES:
        setattr(g, n, nc.dram_tensor(n, W_SHAPES[n], F32, kind="ExternalInput").ap())
    extra_inputs(nc, g)
    g.out = nc.dram_tensor("out", [NLAT, D], F32, kind="ExternalOutput").ap()
    g.R = {'A': ('A', nc.dram_tensor("RA", [D, NT], F32).ap()), 'B': ('B', nc.dram_tensor("RB", [D, NT], F32).ap())}
    extra_scratch(nc, g)
    g.stg_cnt = 0
    if plan is None:
        plan = [('tin', 'A')]
        for l in range(DEPTH):
            plan += [('mix', l, 'A', 'B'), ('ffn', l, 'B', 'A')]
        plan += [('tout', 'A')]
    with ExitStack() as es:
        P = Prog(nc, es)
        NW = 51200
        arena_t = nc.alloc_sbuf_tensor("arena", [128, NW], F32)
        A = Arena(arena_t, NW)
        g.ps = nc.alloc_psum_tensor("ps", [128, 8, 512], F32)
        setup_phase(P, A, g)
        for st in plan:
            if st[0] == 'tin':
                transpose_in(P, A, g, g.R[st[1]])
            elif st[0] == 'tout':
                transpose_out(P, A, g, g.R[st[1]])
            elif st[0] == 'ffn':
                ffn_phase(P, A, g, st[1], g.R[st[2]], g.R[st[3]], st[1] == DEPTH - 1)
            elif st[0] == 'mix':
                mixer_phase(P, A, g, st[1], g.R[st[2]], g.R[st[3]], st[1] == DEPTH - 1)
        P.barrier()
        simulate(P)
        g.counts = P.emit()
    return nc, g


def host_inputs(inp):
    pv = np.zeros((128, NPV), np.float32)
    pv[:, PV['ng']:PV['ng'] + 128] = fm(inp['norm_g'])
    pv[:, PV['bmod']:PV['bmod'] + 192] = fm(inp['b_mod'])
    pv[:, PV['alog']:PV['alog'] + 32] = fm(inp['a_lower_logits'])
    pv[:, PV['aog']:PV['aog'] + 16] = fm(inp['a_out_g'])
    pv[:, PV['cd']:PV['cd'] + 8] = fm(inp['c_d'])
    pv[:, PV['fcw']:PV['fcw'] + 528] = fm(inp['f_conv_w'])
    pv[:, PV['fcb']:PV['fcb'] + 176] = fm(inp['f_conv_b'])
    shared = {'pv': pv, 'ident': np.eye(128, dtype=np.float32)}
    for n in W_NAMES:
        shared[n] = np.ascontiguousarray(np.asarray(inp[n], np.float32))
    shared.update(extra_host(inp))
    maps = []
    for core in range(8):
        b = core % 4
        m = dict(shared)
        m['xin'] = np.ascontiguousarray(np.concatenate([inp['ctx'][b], inp['x'][b]], axis=0).astype(np.float32))
        cv = np.stack([fm(inp['c'][b]), fm(inp['c_ctx'])], axis=-1)
        m['cv'] = np.ascontiguousarray(cv.astype(np.float32))
        maps.append(m)
    return maps


_CACHE = {}


def kernel(**inp):
    if 'nc' not in _CACHE:
        _CACHE['nc'] = build()[0]
    nc = _CACHE['nc']
    maps = host_inputs(inp)
    res = run_bass_kernel_spmd(nc, maps, core_ids=list(range(8)))
    return np.stack([np.asarray(res.results[b]['out'], np.float32) for b in range(4)], axis=0)
```
